# Optimizing a Trainium2 kernel written in Bass

```python
import jax, jax.numpy as jnp
from jax import lax
import numpy as np

D_MODEL = 1024
BATCH = 2
SEQ = 8192
DEPTH = 2

CHUNK = 64
PLE_DIM = 256
N_A_LAYERS = DEPTH // 2
N_B_LAYERS = DEPTH - N_A_LAYERS
N_DENSE = (DEPTH + 1) // 2
N_MOE = DEPTH // 2

RET_HEADS = 4
RET_QK_DIM = D_MODEL // RET_HEADS
RET_V_DIM = 2 * RET_QK_DIM
RET_QK_WIDTH = RET_HEADS * RET_QK_DIM
RET_V_WIDTH = RET_HEADS * RET_V_DIM
ROPE_BASE = 10000.0

ATT_HEADS = 16
ATT_HEAD_DIM = D_MODEL // ATT_HEADS
ATT_WIDTH = ATT_HEADS * ATT_HEAD_DIM
LEFT_CHUNKS = 8
BAND = (LEFT_CHUNKS + 1) * CHUNK
REL_CLIP = 256
N_REL = REL_CLIP + CHUNK

D_FF_DENSE = 2816
N_EXPERTS = 8
TOP_K = 2
D_FF_EXPERT = 3584
EPS = 1e-6

kernel_name = 'yoco_retention_chunkband_moe_ple'


def rmsnorm(x, g):
    xf = x.astype(jnp.float32)
    y = xf * lax.rsqrt(jnp.mean(xf * xf, axis=-1, keepdims=True) + EPS)
    return (y * g.astype(jnp.float32)).astype(x.dtype)


def swiglu(x, w_gate, w_up, w_down):
    return (jax.nn.silu(x @ w_gate) * (x @ w_up)) @ w_down


def rotary(x, positions):
    half = x.shape[-1] // 2
    inv_freq = 1.0 / (ROPE_BASE ** jnp.linspace(0.0, 1.0, half, dtype=jnp.float32))
    ang = positions.astype(jnp.float32)[..., None] * inv_freq
    cos = jnp.cos(ang)[:, :, None, :]
    sin = jnp.sin(ang)[:, :, None, :]
    x1 = x[..., :half].astype(jnp.float32)
    x2 = x[..., half:].astype(jnp.float32)
    return jnp.concatenate([x1 * cos - x2 * sin, x1 * sin + x2 * cos], axis=-1).astype(x.dtype)


def chunkwise_retention(q, k, v):
    B, S, H, dk = q.shape
    dv = v.shape[-1]
    nc = S // CHUNK
    lg = jnp.log1p(-jnp.exp2(-5.0 - jnp.arange(H, dtype=jnp.float32)))
    idx = jnp.arange(CHUNK, dtype=jnp.float32)
    diff = idx[:, None] - idx[None, :]
    causal = diff >= 0
    dmask = jnp.where(causal, jnp.exp(jnp.where(causal, diff, 0.0)[None] * lg[:, None, None]), 0.0)
    xi = jnp.exp((idx[:, None] + 1.0) * lg[None, :])
    zeta = jnp.exp((CHUNK - 1.0 - idx)[:, None] * lg[None, :])
    g_chunk = jnp.exp(CHUNK * lg)

    def to_chunks(t):
        return jnp.moveaxis(t.astype(jnp.float32).reshape(B, nc, CHUNK, H, t.shape[-1]), 1, 0)

    def step(state, inp):
        qn, kn, vn = inp
        s = jnp.einsum('bihd,bjhd->bhij', qn, kn) * dmask
        inner = jnp.einsum('bhij,bjhe->bihe', s, vn)
        cross = jnp.einsum('bihd,bhde->bihe', qn, state) * xi[None, :, :, None]
        state = state * g_chunk[None, :, None, None] + jnp.einsum(
            'bjhd,bjhe->bhde', kn * zeta[None, :, :, None], vn)
        return state, inner + cross

    state0 = jnp.zeros((B, H, dk, dv), jnp.float32)
    _, out = lax.scan(step, state0, (to_chunks(q), to_chunks(k), to_chunks(v)))
    return jnp.moveaxis(out, 0, 1).reshape(B, S, H, dv)


def retention_mixer(xn, positions, w_in, gn_gain, w_o):
    B, S, _ = xn.shape
    proj = xn @ w_in
    q, k, v, g = jnp.split(proj, [RET_QK_WIDTH, 2 * RET_QK_WIDTH, 2 * RET_QK_WIDTH + RET_V_WIDTH], axis=-1)
    q = rotary(q.reshape(B, S, RET_HEADS, RET_QK_DIM), positions)
    k = rotary(k.reshape(B, S, RET_HEADS, RET_QK_DIM), positions) * (RET_QK_DIM ** -0.5)
    v = v.reshape(B, S, RET_HEADS, RET_V_DIM)
    o = chunkwise_retention(q, k, v)
    o = rmsnorm(o, gn_gain.reshape(RET_HEADS, RET_V_DIM)).astype(xn.dtype)
    return (jax.nn.silu(g) * o.reshape(B, S, RET_V_WIDTH)) @ w_o


def shared_band_kv(h, norm_kv, w_kv):
    B, S, _ = h.shape
    kv = rmsnorm(h, norm_kv) @ w_kv
    k, v = jnp.split(kv, 2, axis=-1)
    pad = ((0, 0), (LEFT_CHUNKS * CHUNK, 0), (0, 0), (0, 0))
    k = jnp.pad(k.reshape(B, S, ATT_HEADS, ATT_HEAD_DIM), pad)
    v = jnp.pad(v.reshape(B, S, ATT_HEADS, ATT_HEAD_DIM), pad)
    return k, v


def band_attention_mixer(xn, kp, vp, w_q, rel_table, w_o):
    B, S, _ = xn.shape
    nc = S // CHUNK
    q = (xn @ w_q).reshape(B, nc, CHUNK, ATT_HEADS, ATT_HEAD_DIM) * (ATT_HEAD_DIM ** -0.5)
    q = jnp.moveaxis(q, 1, 0)
    qi = np.arange(CHUNK)[:, None]
    kj = np.arange(BAND)[None, :]
    rel = np.clip(qi - kj + LEFT_CHUNKS * CHUNK, -(CHUNK - 1), REL_CLIP) + (CHUNK - 1)
    bias = rel_table[:, rel].astype(jnp.float32)
    k_offset = jnp.arange(BAND) - LEFT_CHUNKS * CHUNK

    def one_chunk(args):
        c, q_c = args
        start = c * CHUNK
        k_band = lax.dynamic_slice_in_dim(kp, start, BAND, axis=1)
        v_band = lax.dynamic_slice_in_dim(vp, start, BAND, axis=1)
        s = jnp.einsum('bihd,bjhd->bhij', q_c, k_band).astype(jnp.float32) + bias
        valid = (start + k_offset) >= 0
        s = jnp.where(valid, s, -jnp.inf)
        a = jax.nn.softmax(s, axis=-1).astype(v_band.dtype)
        return jnp.einsum('bhij,bjhd->bihd', a, v_band)

    o = lax.map(one_chunk, (jnp.arange(nc), q))
    o = jnp.moveaxis(o, 0, 1).reshape(B, S, ATT_WIDTH)
    return o @ w_o


def moe_swiglu(xn, w_router, w_gate, w_up, w_down):
    logits = (xn @ w_router).astype(jnp.float32)
    top_v, top_i = lax.top_k(logits, TOP_K)
    top_w = jax.nn.softmax(top_v, axis=-1)
    gates = jnp.sum(jax.nn.one_hot(top_i, N_EXPERTS, dtype=jnp.float32) * top_w[..., None], axis=-2)
    gates = gates.astype(xn.dtype)
    y = jnp.zeros_like(xn)
    for e in range(N_EXPERTS):
        y = y + gates[..., e:e + 1] * swiglu(xn, w_gate[e], w_up[e], w_down[e])
    return y


def per_layer_embedding(h, p_i, g, w_up, w_gate):
    gate = jax.nn.sigmoid(rmsnorm(h, g) @ w_gate)
    return (p_i @ w_up) * gate


def setup_inputs(seed: int = 0) -> dict:
    key = jax.random.key(seed)
    ks = jax.random.split(key, 26)
    f32 = jnp.float32

    def nrm(k, shape, fan_in):
        return jax.random.normal(k, shape, f32) * (fan_in ** -0.5)

    def gain(k, shape):
        return 1.0 + 0.05 * jax.random.normal(k, shape, f32)

    offsets = jax.random.randint(ks[2], (BATCH, 1), 0, 4096, dtype=jnp.int32)
    positions = offsets + jnp.arange(SEQ, dtype=jnp.int32)[None, :]
    return {
        'x': jax.random.normal(ks[0], (BATCH, SEQ, D_MODEL), f32),
        'p': jax.random.normal(ks[1], (DEPTH, BATCH, SEQ, PLE_DIM), f32),
        'positions': positions,
        'norm_mix': gain(ks[3], (DEPTH, D_MODEL)),
        'norm_ffn': gain(ks[4], (DEPTH, D_MODEL)),
        'norm_ple': gain(ks[5], (DEPTH, D_MODEL)),
        'w_in_a': nrm(ks[6], (N_A_LAYERS, D_MODEL, 2 * RET_QK_WIDTH + 2 * RET_V_WIDTH), D_MODEL),
        'ret_gn': gain(ks[7], (N_A_LAYERS, RET_V_WIDTH)),
        'w_out_a': nrm(ks[8], (N_A_LAYERS, RET_V_WIDTH, D_MODEL), RET_V_WIDTH),
        'norm_kv': gain(ks[9], (D_MODEL,)),
        'w_kv': nrm(ks[10], (D_MODEL, 2 * ATT_WIDTH), D_MODEL),
        'w_q_b': nrm(ks[11], (N_B_LAYERS, D_MODEL, ATT_WIDTH), D_MODEL),
        'rel_bias': 0.2 * jax.random.normal(ks[12], (N_B_LAYERS, ATT_HEADS, N_REL), f32),
        'w_out_b': nrm(ks[13], (N_B_LAYERS, ATT_WIDTH, D_MODEL), ATT_WIDTH),
        'w_gate_dense': nrm(ks[14], (N_DENSE, D_MODEL, D_FF_DENSE), D_MODEL),
        'w_up_dense': nrm(ks[15], (N_DENSE, D_MODEL, D_FF_DENSE), D_MODEL),
        'w_down_dense': nrm(ks[16], (N_DENSE, D_FF_DENSE, D_MODEL), D_FF_DENSE),
        'w_router': nrm(ks[17], (N_MOE, D_MODEL, N_EXPERTS), D_MODEL),
        'w_gate_moe': nrm(ks[18], (N_MOE, N_EXPERTS, D_MODEL, D_FF_EXPERT), D_MODEL),
        'w_up_moe': nrm(ks[19], (N_MOE, N_EXPERTS, D_MODEL, D_FF_EXPERT), D_MODEL),
        'w_down_moe': nrm(ks[20], (N_MOE, N_EXPERTS, D_FF_EXPERT, D_MODEL), D_FF_EXPERT),
        'w_ple_up': nrm(ks[21], (DEPTH, PLE_DIM, D_MODEL), PLE_DIM),
        'w_ple_gate': nrm(ks[22], (DEPTH, D_MODEL, D_MODEL), D_MODEL),
        'norm_final': gain(ks[23], (D_MODEL,)),
    }


def reference(x, p, positions, norm_mix, norm_ffn, norm_ple, w_in_a, ret_gn, w_out_a,
              norm_kv, w_kv, w_q_b, rel_bias, w_out_b, w_gate_dense, w_up_dense, w_down_dense,
              w_router, w_gate_moe, w_up_moe, w_down_moe, w_ple_up, w_ple_gate, norm_final):
    h = x
    kp = None
    vp = None
    for i in range(DEPTH):
        xn = rmsnorm(h, norm_mix[i])
        if i < N_A_LAYERS:
            h = h + retention_mixer(xn, positions, w_in_a[i], ret_gn[i], w_out_a[i])
        else:
            if i == N_A_LAYERS:
                kp, vp = shared_band_kv(h, norm_kv, w_kv)
            j = i - N_A_LAYERS
            h = h + band_attention_mixer(xn, kp, vp, w_q_b[j], rel_bias[j], w_out_b[j])
        xn = rmsnorm(h, norm_ffn[i])
        m = i // 2
        if i % 2 == 0:
            h = h + swiglu(xn, w_gate_dense[m], w_up_dense[m], w_down_dense[m])
        else:
            h = h + moe_swiglu(xn, w_router[m], w_gate_moe[m], w_up_moe[m], w_down_moe[m])
        h = h + per_layer_embedding(h, p[i], norm_ple[i], w_ple_up[i], w_ple_gate[i])
    return rmsnorm(h, norm_final)
```

```python
import contextlib
import numpy as np
import ml_dtypes
import concourse.bass as bass
import concourse.mybir as mybir
from concourse.bass_utils import run_bass_kernel_spmd

F32 = mybir.dt.float32
BF16 = mybir.dt.bfloat16
I32 = mybir.dt.int32
AF = mybir.ActivationFunctionType
ALU = mybir.AluOpType

NCORES = 8
T = 2048
TT = 1024
D = 1024
EPS = 1e-6
GAM = [1.0 - 2.0 ** (-5 - h) for h in range(4)]
RC = 128
NEG = -30000.0
TWO_PI = 2.0 * np.pi
CW1 = 6.28125
CW2 = TWO_PI - CW1

ENGS = ("pe", "act", "dve", "pool", "sp")


class Sched:
    def __init__(self, nc):
        self.nc = nc
        self.ops = []
        self.last_w = {}
        self.readers = {}
        self.barrier_idx = None
        self.last_eng = {}
        self.last_dsem = {}

    def add(self, eng, fn, reads=(), writes=(), dsem=None):
        idx = len(self.ops)
        deps = {}
        for k in reads:
            w = self.last_w.get(k)
            if w is not None:
                deps[w] = True
        for k in writes:
            w = self.last_w.get(k)
            if w is not None:
                deps[w] = True
            for r in self.readers.get(k, ()):
                if r not in deps:
                    deps[r] = False
        for k in reads:
            self.readers.setdefault(k, []).append(idx)
        for k in writes:
            self.last_w[k] = idx
            self.readers[k] = []
        if self.barrier_idx is not None:
            deps[self.barrier_idx] = True
        deps.pop(idx, None)
        self.ops.append(dict(eng=eng, fn=fn, deps=deps, dsem=dsem, sig=False, val=None))
        if dsem is None:
            self.last_eng[eng] = idx
        else:
            self.last_dsem[dsem] = idx
        return idx

    def barrier(self, fn):
        idx = len(self.ops)
        deps = {}
        for e, i in self.last_eng.items():
            deps[i] = True
        for n, i in self.last_dsem.items():
            deps[i] = True
        self.ops.append(dict(eng="dve", fn=fn, deps=deps, dsem=None, sig=False, val=None))
        self.last_eng["dve"] = idx
        self.barrier_idx = idx
        self.last_w = {}
        self.readers = {}
        return idx

    def emit(self, final_wait_dsems=()):
        nc = self.nc
        ops = self.ops
        for i, op in enumerate(ops):
            for d, strong in op["deps"].items():
                p = ops[d]
                if p["dsem"] is not None:
                    continue
                if p["eng"] == op["eng"] and (op["eng"] == "pe" or not strong):
                    continue
                p["sig"] = True
        cnt = {e: 0 for e in ENGS}
        dcnt = {}
        for op in ops:
            if op["dsem"] is not None:
                dcnt[op["dsem"]] = dcnt.get(op["dsem"], 0) + 16
                op["val"] = dcnt[op["dsem"]]
            elif op["sig"]:
                cnt[op["eng"]] += 1
                op["val"] = cnt[op["eng"]]
        dsem_names = sorted(dcnt.keys())
        with contextlib.ExitStack() as st:
            esem = {e: st.enter_context(nc.semaphore("s_" + e)) for e in ENGS}
            dsem = {n: st.enter_context(nc.semaphore("d_%d" % j)) for j, n in enumerate(dsem_names)}
            block = st.enter_context(nc.Block())

            def run_engine(ename, eng):
                waited = {}
                for op in ops:
                    if op["eng"] != ename:
                        continue
                    need = {}
                    for d, strong in op["deps"].items():
                        p = ops[d]
                        if p["dsem"] is not None:
                            key = ("d", p["dsem"])
                        else:
                            if p["eng"] == ename and (ename == "pe" or not strong):
                                continue
                            key = ("e", p["eng"])
                        if p["val"] > need.get(key, 0):
                            need[key] = p["val"]
                    for key, v in need.items():
                        if waited.get(key, 0) >= v:
                            continue
                        waited[key] = v
                        s = dsem[key[1]] if key[0] == "d" else esem[key[1]]
                        eng.wait_ge(s, v)
                    ins = op["fn"](eng)
                    if op["dsem"] is not None:
                        ins.then_inc(dsem[op["dsem"]], 16)
                    elif op["sig"]:
                        ins.then_inc(esem[ename], 1)
                if ename == "sp":
                    for n in final_wait_dsems:
                        if n in dcnt:
                            eng.wait_ge(dsem[n], dcnt[n])

            @block.tensor
            def _(e):
                run_engine("pe", e)

            @block.scalar
            def _(e):
                run_engine("act", e)

            @block.vector
            def _(e):
                run_engine("dve", e)

            @block.gpsimd
            def _(e):
                run_engine("pool", e)

            @block.sync
            def _(e):
                run_engine("sp", e)


ARENA_BASE = 18432
ARENA_END = 229376
VEC_MIX, VEC_FFN, VEC_PLE, VEC_KV, VEC_FIN, VEC_GN = 0, 16, 32, 48, 56, 64
NVEC = 80
SLAB_KV = 8 * 512 + 4 * 1040


class Builder:
    def __init__(self, stage):
        self.stage = stage
        nc = bass.Bass("TRN2", target_bir_lowering=False)
        self.nc = nc
        self.S = Sched(nc)
        self.off = ARENA_BASE
        self.uid = 0
        self.ps = [nc.alloc_psum_tensor("psum%d" % i, [128, 1024], F32) for i in range(4)]
        self.sing = list(range(8))
        self.pairs = []
        self.si = 0
        self.pi = 0
        self.wi = 0
        self.final_dsems = []

    def sb(self, name, shape, dt):
        esz = 2 if dt == BF16 else 4
        n = int(np.prod(shape[1:])) * esz
        n = (n + 63) // 64 * 64
        assert self.off + n <= ARENA_END, ("SBUF overflow", name, self.off + n - ARENA_END)
        self.uid += 1
        t = self.nc.alloc_sbuf_tensor_at("%s_%d" % (name, self.uid), list(shape), dt, offset=self.off)
        self.off += n
        return t

    def sb_at(self, name, shape, dt, offset):
        self.uid += 1
        return self.nc.alloc_sbuf_tensor_at("%s_%d" % (name, self.uid), list(shape), dt, offset=offset)

    def dram_in(self, name, shape, dt):
        return self.nc.dram_tensor(name, list(shape), dt, kind="ExternalInput").ap()

    def dram_out(self, name, shape, dt):
        return self.nc.dram_tensor(name, list(shape), dt, kind="ExternalOutput").ap()

    def dram_int(self, name, shape, dt):
        return self.nc.dram_tensor(name, list(shape), dt).ap()

    def set_banks(self, singles, pairs):
        self.sing = list(singles)
        self.pairs = list(pairs)
        self.si = 0
        self.pi = 0

    def bank(self):
        b = self.sing[self.si % len(self.sing)]
        self.si += 1
        return self.ps[b // 2][:, (b % 2) * 512:(b % 2) * 512 + 512], ("ps", b)

    def bank2(self):
        p = self.pairs[self.pi % len(self.pairs)]
        self.pi += 1
        return self.ps[p], [("ps", 2 * p), ("ps", 2 * p + 1)]

    def op(self, eng, fn, reads=(), writes=(), dsem=None):
        return self.S.add(eng, fn, reads, writes, dsem)

    def mm(self, out, lhsT, rhs, start, stop, reads, writes):
        self.op("pe", lambda e: e.matmul(out, lhsT=lhsT, rhs=rhs, start=start, stop=stop), reads, writes)

    def wload(self, dst, src, key):
        self.op("pool", lambda e: e.dma_start(out=dst, in_=src), writes=[key], dsem="w%d" % key[1])

    def wslot(self, shape):
        i = self.wi % self.nslots
        self.wi += 1
        a, b = shape
        assert a * b <= 4096
        v = self.wslots[i][:, 0:a * b].rearrange("p (a b) -> p a b", a=a)
        return v, ("w", i)

    def phase_barrier(self):
        bt = self.bar_t
        self.S.barrier(lambda e: e.memset(bt[:], 0.0))

    def setup_common(self):
        nc = self.nc
        self.vecs_d = self.dram_in("vecs", [128, NVEC], F32)
        self.consts_d = self.dram_in("consts", [128, 4 * 128 + 4 * 128 + 4 + 128 + 1], F32)
        self.coef_d = self.dram_in("coef", [128, 40], F32)
        self.hT = self.sb("hT", [128, 8, T], F32)
        self.vecs = self.sb("vecs", [128, NVEC], F32)
        self.consts = self.sb("consts", [128, 1157], F32)
        self.coef = self.sb("coef", [128, 40], F32)
        self.ident = self.sb("ident", [128, 128], BF16)
        self.ones = self.sb("ones", [128, 128], BF16)
        self.bar_t = self.sb("bar", [128, 16], F32)
        self.Dm = self.consts[:, 0:512].rearrange("p (h i) -> p h i", h=4)
        self.xi = self.consts[:, 512:1024].rearrange("p (h i) -> p h i", h=4)
        self.zeta = self.consts[:, 1024:1028]
        self.identf = self.consts[:, 1028:1156]
        self.invf = self.consts[:, 1156:1157]
        self.persist_end = self.off
        op = self.op
        op("sp", lambda e: e.dma_start(out=self.vecs[:], in_=self.vecs_d), writes=["vecs"], dsem="c0")
        op("sp", lambda e: e.dma_start(out=self.consts[:], in_=self.consts_d), writes=["consts"], dsem="c1")
        op("sp", lambda e: e.dma_start(out=self.coef[:], in_=self.coef_d), writes=["coef"], dsem="c2")
        op("dve", lambda e: e.tensor_copy(out=self.ident[:], in_=self.identf), reads=["consts"], writes=["ident"])
        op("dve", lambda e: e.memset(self.ones[:], 1.0), writes=["ones"])

    def load_h(self, src):
        for k in range(8):
            self.op("sp", lambda e, k=k: e.dma_start(out=self.hT[:, k, :], in_=src[:, k, :]),
                    writes=[("h", k, hf, tb) for hf in range(2) for tb in range(2)], dsem="hl%d" % k)

    def store_h(self, dst, name):
        for k in range(8):
            self.op("sp", lambda e, k=k: e.dma_start(out=dst[:, k, :], in_=self.hT[:, k, :]),
                    reads=[("h", k, hf, tb) for hf in range(2) for tb in range(2)], writes=[(name, k)],
                    dsem="hs%d" % (k % 4))
        self.final_dsems += ["hs%d" % i for i in range(4)]

    def alloc_norm(self):
        self.sq = self.sb("sq", [128, 8, 512], BF16)
        self.rstd = self.sb("rstd", [128, TT], F32)

    def rmsnorm(self, hf, gcol, dst_fn, dst_key_fn):
        op = self.op
        hT, sq, rstd, vecs, ones = self.hT, self.sq, self.rstd, self.vecs, self.ones
        for tb in range(2):
            c0 = hf * TT + tb * 512
            op("act", lambda e, c0=c0: e.activation(out=sq[:], in_=hT[:, :, c0:c0 + 512], func=AF.Square),
               reads=[("h", k, hf, tb) for k in range(8)], writes=["sq"])
            bk, bkey = self.bank()
            for k in range(8):
                self.mm(bk, ones[:], sq[:, k, :], k == 0, k == 7, ["sq", "ones"], [bkey])
            rs = rstd[:, tb * 512:(tb + 1) * 512]
            op("act", lambda e, bk=bk, rs=rs: e.activation(out=rs, in_=bk, func=AF.Sqrt, scale=1.0 / D, bias=EPS),
               reads=[bkey], writes=[("rstd", tb)])
            op("dve", lambda e, rs=rs: e.reciprocal(out=rs, in_=rs), reads=[("rstd", tb)], writes=[("rstd", tb)])
            for k in range(8):
                dst = dst_fn(k, tb)
                op("dve", lambda e, k=k, c0=c0, dst=dst, rs=rs: e.scalar_tensor_tensor(
                    out=dst, in0=hT[:, k, c0:c0 + 512], scalar=vecs[:, gcol + k:gcol + k + 1], in1=rs,
                    op0=ALU.mult, op1=ALU.mult),
                   reads=[("h", k, hf, tb), ("rstd", tb), "vecs"], writes=[dst_key_fn(k, tb)])

    def norm_to_xn(self, hf, gcol):
        xn = self.xn
        self.rmsnorm(hf, gcol, lambda k, tb: xn[:, k, tb * 512:(tb + 1) * 512], lambda k, tb: ("xn", k, tb))

    def h_add(self, bk, bkey, n, hf, tb):
        hT = self.hT
        c0 = hf * TT + tb * 512
        self.op("dve", lambda e: e.tensor_tensor(out=hT[:, n, c0:c0 + 512], in0=bk, in1=hT[:, n, c0:c0 + 512], op=ALU.add),
                reads=[bkey, ("h", n, hf, tb)], writes=[("h", n, hf, tb)])

    def rotary_tables(self, hf):
        op = self.op
        cos, sin = self.cos, self.sin
        t0, t1, t2 = self.rt[0], self.rt[1], self.rt[2]
        ti = self.rt_i
        c0 = hf * TT
        pos_d = self.pos_d
        op("sp", lambda e: e.dma_start(out=ti[:], in_=pos_d[0:1, c0:c0 + TT].broadcast_to([128, TT])),
           writes=["rt_i"], dsem="pos")
        op("dve", lambda e: e.tensor_copy(out=t0[:], in_=ti[:]), reads=["rt_i"], writes=["rt0"])
        op("dve", lambda e: e.tensor_scalar(out=t0[:], in0=t0[:], scalar1=self.invf, scalar2=None, op0=ALU.mult),
           reads=["rt0", "consts"], writes=["rt0"])
        op("dve", lambda e: e.tensor_scalar(out=t1[:], in0=t0[:], scalar1=float(1.0 / TWO_PI), scalar2=None, op0=ALU.mult),
           reads=["rt0"], writes=["rt1"])
        op("dve", lambda e: e.tensor_copy(out=ti[:], in_=t1[:]), reads=["rt1"], writes=["rt_i"])
        op("dve", lambda e: e.tensor_copy(out=t1[:], in_=ti[:]), reads=["rt_i"], writes=["rt1"])
        op("dve", lambda e: e.scalar_tensor_tensor(out=t0[:], in0=t1[:], scalar=-CW1, in1=t0[:], op0=ALU.mult, op1=ALU.add),
           reads=["rt0", "rt1"], writes=["rt0"])
        op("dve", lambda e: e.scalar_tensor_tensor(out=t0[:], in0=t1[:], scalar=-CW2, in1=t0[:], op0=ALU.mult, op1=ALU.add),
           reads=["rt0", "rt1"], writes=["rt0"])
        for (shift, dst, dkey) in ((0.0, sin, "sin"), (0.5 * np.pi, cos, "cos")):
            if shift != 0.0:
                op("dve", lambda e, shift=shift: e.tensor_scalar(out=t0[:], in0=t0[:], scalar1=float(shift), scalar2=None, op0=ALU.add),
                   reads=["rt0"], writes=["rt0"])
            op("dve", lambda e: e.tensor_scalar(out=t1[:], in0=t0[:], scalar1=float(np.pi), scalar2=None, op0=ALU.is_gt),
               reads=["rt0"], writes=["rt1"])
            op("dve", lambda e: e.scalar_tensor_tensor(out=t2[:], in0=t1[:], scalar=-TWO_PI, in1=t0[:], op0=ALU.mult, op1=ALU.add),
               reads=["rt0", "rt1"], writes=["rt2"])
            op("dve", lambda e: e.tensor_scalar(out=t1[:], in0=t2[:], scalar1=float(-np.pi), scalar2=None, op0=ALU.is_lt),
               reads=["rt2"], writes=["rt1"])
            op("dve", lambda e: e.scalar_tensor_tensor(out=t2[:], in0=t1[:], scalar=TWO_PI, in1=t2[:], op0=ALU.mult, op1=ALU.add),
               reads=["rt2", "rt1"], writes=["rt2"])
            op("act", lambda e, dst=dst: e.activation(out=dst[:], in_=t2[:], func=AF.Sin), reads=["rt2"], writes=[dkey])

    def proj_rot(self, wv, wkey, dst, dkey, cs, sn, cskeys):
        op = self.op
        xn = self.xn
        tmp = self.sq
        for tb in range(2):
            cs_ = cs[:, tb * 512:(tb + 1) * 512]
            sn_ = sn[:, tb * 512:(tb + 1) * 512]
            b1, k1 = self.bank()
            b2, k2 = self.bank()
            for k in range(8):
                self.mm(b1, wv[:, k, 0:128], xn[:, k, tb * 512:(tb + 1) * 512], k == 0, k == 7, [wkey, ("xn", k, tb)], [k1])
            for k in range(8):
                self.mm(b2, wv[:, k, 128:256], xn[:, k, tb * 512:(tb + 1) * 512], k == 0, k == 7, [wkey, ("xn", k, tb)], [k2])
            ta, tbb = self.rtmp[0], self.rtmp[1]
            op("dve", lambda e, b1=b1, cs_=cs_: e.tensor_tensor(out=ta[:], in0=b1, in1=cs_, op=ALU.mult),
               reads=[k1] + cskeys, writes=["rtmp0"])
            op("dve", lambda e, b2=b2, sn_=sn_: e.tensor_tensor(out=tbb[:], in0=b2, in1=sn_, op=ALU.mult),
               reads=[k2] + cskeys, writes=["rtmp1"])
            d0 = dst[:, 0, tb * 512:(tb + 1) * 512]
            op("dve", lambda e, d0=d0: e.tensor_tensor(out=d0, in0=ta[:], in1=tbb[:], op=ALU.subtract),
               reads=["rtmp0", "rtmp1"], writes=[(dkey, 0, tb)])
            op("dve", lambda e, b1=b1, sn_=sn_: e.tensor_tensor(out=ta[:], in0=b1, in1=sn_, op=ALU.mult),
               reads=[k1] + cskeys, writes=["rtmp0"])
            op("dve", lambda e, b2=b2, cs_=cs_: e.tensor_tensor(out=tbb[:], in0=b2, in1=cs_, op=ALU.mult),
               reads=[k2] + cskeys, writes=["rtmp1"])
            d1 = dst[:, 1, tb * 512:(tb + 1) * 512]
            op("dve", lambda e, d1=d1: e.tensor_tensor(out=d1, in0=ta[:], in1=tbb[:], op=ALU.add),
               reads=["rtmp0", "rtmp1"], writes=[(dkey, 1, tb)])

    def make_ktok(self, h):
        op = self.op
        kT, ktok, ident = self.kT, self.ktok, self.ident
        for tl in range(8):
            bk, bkey = self.bank()
            bb = bk[:, 0:128].bitcast(BF16)
            tb = tl // 4
            for dc in range(2):
                self.op("pe", lambda e, dc=dc, tl=tl, bb=bb: e.transpose(bb[:, dc * 128:(dc + 1) * 128], kT[:, dc, tl * 128:(tl + 1) * 128], ident[:]),
                        reads=[("kT", dc, tb), "ident"], writes=[bkey])
            op("act", lambda e, bb=bb, tl=tl: e.activation(out=ktok[:, tl, :], in_=bb, func=AF.Copy, scale=self.zeta[:, h:h + 1]),
               reads=[bkey, "consts"], writes=[("ktok", tl)])

    def proj_v(self, wv, wkey):
        xn, vtok = self.xn, self.vtok
        for tl in range(8):
            tb = tl // 4
            bk, bkey = self.bank()
            for k in range(8):
                self.mm(bk, xn[:, k, tl * 128:(tl + 1) * 128], wv[:, k, :], k == 0, k == 7, [wkey, ("xn", k, tb)], [bkey])
            self.op("act", lambda e, bk=bk, tl=tl: e.activation(out=vtok[:, tl, :], in_=bk, func=AF.Copy),
                    reads=[bkey], writes=[("vtok", tl)])

    def state_update(self, h, tl):
        ktok, vtok, st = self.ktok, self.vtok, self.state
        b2, keys = self.bank2()
        for dc in range(2):
            self.mm(b2[:, dc * 512:(dc + 1) * 512], ktok[:, tl, dc * 128:(dc + 1) * 128], vtok[:, tl, :], True, True,
                    [("ktok", tl), ("vtok", tl)], [keys[dc]])
        g = float(GAM[h] ** RC)
        sv = st[:, h, :]
        self.op("dve", lambda e: e.scalar_tensor_tensor(out=sv, in0=sv, scalar=g, in1=b2[:, :], op0=ALU.mult, op1=ALU.add),
                reads=keys + [("state", h)], writes=[("state", h)])

    def alloc_l0mix(self, main):
        self.nslots = 3 if main else 4
        self.wslots = [self.sb("wslot", [128, 4096], BF16) for _ in range(self.nslots)]
        self.alloc_norm()
        self.xn = self.sb("xn", [128, 8, TT], BF16)
        self.cos = self.sb("cos", [128, TT], F32)
        self.sin = self.sb("sin", [128, TT], F32)
        self.rtmp = [self.sb("rtmp", [128, 512], F32) for _ in range(2)]
        self.kT = self.sb("kT", [128, 2, TT], BF16)
        self.ktok = self.sb("ktok", [128, 8, 256], BF16)
        self.vtok = self.sb("vtok", [128, 8, 512], BF16)
        self.state = self.sb("state", [128, 4, 1024], F32)
        if main:
            self.cosq = self.sb("cosq", [128, TT], F32)
            self.sinq = self.sb("sinq", [128, TT], F32)
            self.qxT = self.sb("qxT", [128, 2, TT], BF16)
            g_off = self.off
            self.gT = self.sb("gT", [128, 4, TT], BF16)
            o_off = self.off
            self.oT = self.sb("oT", [128, 4, TT], BF16)
            self.state_bf = self.sb("state_bf", [128, 1024], BF16)
            self.PT = [self.sb("PT", [128, 128], BF16) for _ in range(2)]
            self.osq = [self.sb("osq", [128, 4, 128], BF16) for _ in range(2)]
            self.msq = self.sb("msq", [128, TT], F32)
            self.rt = [self.sb_at("rt0", [128, TT], F32, g_off), self.sb_at("rt1", [128, TT], F32, g_off + 4096),
                       self.sb_at("rt2", [128, TT], F32, o_off)]
            self.rt_i = self.sb_at("rt_i", [128, TT], I32, o_off + 4096)
        else:
            self.rt = [self.rtmp_big(i) for i in range(3)]
            self.rt_i = self.sb("rt_i", [128, TT], I32)
        print("l0mix alloc end", self.off, ARENA_END - self.off)

    def rtmp_big(self, i):
        return self.sb("rtbig%d" % i, [128, TT], F32)

    def l0_pass1(self):
        op = self.op
        mark = self.off
        self.alloc_l0mix(main=False)
        self.set_banks([0, 1, 2, 3], [2, 3])
        st = self.state
        op("dve", lambda e: e.memset(st[:], 0.0), writes=[("state", h) for h in range(4)])
        w_in = self.w_in
        for hf in range(2):
            self.rotary_tables(hf)
            self.norm_to_xn(hf, VEC_MIX)
            for h in range(4):
                wk, kk = self.wslot((8, 256))
                self.wload(wk, w_in[:, :, 1024 + h * 256:1024 + (h + 1) * 256], kk)
                wv, kv = self.wslot((8, 512))
                self.wload(wv, w_in[:, :, 2048 + h * 512:2048 + (h + 1) * 512], kv)
                self.proj_rot(wk, kk, self.kT, "kT", self.cos, self.sin, ["cos", "sin"])
                self.make_ktok(h)
                self.proj_v(wv, kv)
                for tl in range(8):
                    self.state_update(h, tl)
        return mark

    def l0_combine(self, sall):
        op = self.op
        st = self.state
        tmpb = [self.rt[0], self.rt[1]]
        op("dve", lambda e: e.memset(st[:], 0.0), writes=[("state", h) for h in range(4)])
        i = 0
        for src in range(8):
            for h in range(4):
                tb_ = tmpb[i % 2]
                key = ("cmb", i % 2)
                op("sp", lambda e, tb_=tb_, src=src, h=h: e.dma_start(out=tb_[:], in_=sall[src * 128:(src + 1) * 128, h * 1024:(h + 1) * 1024]),
                   writes=[key], dsem="cmb%d" % (i % 2))
                sv = st[:, h, :]
                op("dve", lambda e, tb_=tb_, sv=sv, src=src, h=h: e.scalar_tensor_tensor(
                    out=sv, in0=tb_[:], scalar=self.coef[:, src * 4 + h:src * 4 + h + 1], in1=sv, op0=ALU.mult, op1=ALU.add),
                   reads=[key, ("state", h), "coef"], writes=[("state", h)])
                i += 1

    def l0_main(self):
        op = self.op
        w_in, w_o = self.w_in, self.w_o_a
        self.set_banks([0, 1, 2, 3], [2, 3])
        xn, hT = self.xn, self.hT
        for hf in range(2):
            self.phase_barrier()
            self.rotary_tables(hf)
            self.phase_barrier()
            self.norm_to_xn(hf, VEC_MIX)
            for h in range(4):
                wq, kq = self.wslot((8, 256))
                self.wload(wq, w_in[:, :, h * 256:(h + 1) * 256], kq)
                wk, kk = self.wslot((8, 256))
                self.wload(wk, w_in[:, :, 1024 + h * 256:1024 + (h + 1) * 256], kk)
                xi_b = self.xi[:, h, :].unsqueeze(1).broadcast_to([128, TT // RC, RC])
                op("dve", lambda e, xi_b=xi_b: e.tensor_tensor(out=self.cosq[:].rearrange("p (a b) -> p a b", b=RC),
                                                               in0=self.cos[:].rearrange("p (a b) -> p a b", b=RC), in1=xi_b, op=ALU.mult),
                   reads=["cos", "consts"], writes=["cosq"])
                op("dve", lambda e, xi_b=xi_b: e.tensor_tensor(out=self.sinq[:].rearrange("p (a b) -> p a b", b=RC),
                                                               in0=self.sin[:].rearrange("p (a b) -> p a b", b=RC), in1=xi_b, op=ALU.mult),
                   reads=["sin", "consts"], writes=["sinq"])
                self.proj_rot(wq, kq, self.qxT, "qxT", self.cosq, self.sinq, ["cosq", "sinq"])
                self.proj_rot(wk, kk, self.kT, "kT", self.cos, self.sin, ["cos", "sin"])
                self.make_ktok(h)
                wv, kv = self.wslot((8, 512))
                self.wload(wv, w_in[:, :, 2048 + h * 512:2048 + (h + 1) * 512], kv)
                self.proj_v(wv, kv)
                wg, kg = self.wslot((8, 512))
                self.wload(wg, w_in[:, :, 4096 + h * 512:4096 + (h + 1) * 512], kg)
                gT = self.gT
                for ec in range(4):
                    for tb in range(2):
                        bk, bkey = self.bank()
                        for k in range(8):
                            self.mm(bk, wg[:, k, ec * 128:(ec + 1) * 128], xn[:, k, tb * 512:(tb + 1) * 512], k == 0, k == 7,
                                    [kg, ("xn", k, tb)], [bkey])
                        op("act", lambda e, bk=bk, ec=ec, tb=tb: e.activation(out=gT[:, ec, tb * 512:(tb + 1) * 512], in_=bk, func=AF.Silu),
                           reads=[bkey], writes=[("gT", ec, tb)])
                sbf = self.state_bf
                op("act", lambda e, h=h: e.activation(out=sbf[:], in_=self.state[:, h, :], func=AF.Copy),
                   reads=[("state", h)], writes=["state_bf"])
                pend = None
                for n in range(8):
                    tb = n // 4
                    c0 = n * RC
                    PT = self.PT[n % 2]
                    osq = self.osq[n % 2]
                    bA, kA = self.bank()
                    for dc in range(2):
                        self.mm(bA[:, 0:128], self.kT[:, dc, c0:c0 + RC], self.qxT[:, dc, c0:c0 + RC], dc == 0, dc == 1,
                                [("kT", dc, tb), ("qxT", dc, tb)], [kA])
                    b2, keys2 = self.bank2()
                    for dc in range(2):
                        self.mm(b2[:, dc * 512:(dc + 1) * 512], self.ktok[:, n, dc * 128:(dc + 1) * 128], self.vtok[:, n, :], True, True,
                                [("ktok", n), ("vtok", n)], [keys2[dc]])
                    op("dve", lambda e, bA=bA, PT=PT, h=h: e.tensor_tensor(out=PT[:], in0=bA[:, 0:128], in1=self.Dm[:, h, :], op=ALU.mult),
                       reads=[kA, "consts"], writes=[("PT", n % 2)])
                    bO, kO = self.bank()
                    for ec in range(4):
                        oc = bO[:, ec * 128:(ec + 1) * 128]
                        self.mm(oc, self.vtok[:, n, ec * 128:(ec + 1) * 128], PT[:], True, False, [("vtok", n), ("PT", n % 2)], [kO])
                        for dc in range(2):
                            self.mm(oc, sbf[:, dc * 512 + ec * 128:dc * 512 + (ec + 1) * 128], self.qxT[:, dc, c0:c0 + RC], False, dc == 1,
                                    ["state_bf", ("qxT", dc, tb)], [kO])
                    oTv = self.oT[:, :, c0:c0 + RC]
                    bOv = bO.rearrange("p (a b) -> p a b", a=4)
                    op("act", lambda e, oTv=oTv, bOv=bOv: e.activation(out=oTv, in_=bOv, func=AF.Copy),
                       reads=[kO], writes=[("oT", ec, tb) for ec in range(4)])
                    op("act", lambda e, osq=osq, bOv=bOv: e.activation(out=osq[:], in_=bOv, func=AF.Square),
                       reads=[kO], writes=[("osq", n % 2)])
                    if pend is not None:
                        self.norm_sumsq(*pend)
                    pend = (osq, ("osq", n % 2), c0)
                    g = float(GAM[h] ** RC)
                    sv = self.state[:, h, :]
                    op("dve", lambda e, sv=sv, g=g, b2=b2: e.scalar_tensor_tensor(out=sv, in0=sv, scalar=g, in1=b2[:, :], op0=ALU.mult, op1=ALU.add),
                       reads=keys2 + [("state", h)], writes=[("state", h)])
                    op("act", lambda e, sv=sv: e.activation(out=sbf[:], in_=sv, func=AF.Copy),
                       reads=[("state", h)], writes=["state_bf"])
                self.norm_sumsq(*pend)
                msq, oT = self.msq, self.oT
                op("act", lambda e: e.activation(out=msq[:], in_=msq[:], func=AF.Sqrt, scale=1.0 / 512.0, bias=EPS),
                   reads=["msq"], writes=["msq"])
                op("dve", lambda e: e.reciprocal(out=msq[:], in_=msq[:]), reads=["msq"], writes=["msq"])
                for ec in range(4):
                    gcol = VEC_GN + h * 4 + ec
                    op("dve", lambda e, ec=ec, gcol=gcol: e.scalar_tensor_tensor(
                        out=oT[:, ec, :], in0=oT[:, ec, :], scalar=self.vecs[:, gcol:gcol + 1], in1=msq[:], op0=ALU.mult, op1=ALU.mult),
                       reads=[("oT", ec, 0), ("oT", ec, 1), "msq", "vecs"], writes=[("oT", ec, 0), ("oT", ec, 1)])
                    op("dve", lambda e, ec=ec: e.tensor_tensor(out=oT[:, ec, :], in0=oT[:, ec, :], in1=gT[:, ec, :], op=ALU.mult),
                       reads=[("oT", ec, 0), ("oT", ec, 1), ("gT", ec, 0), ("gT", ec, 1)], writes=[("oT", ec, 0), ("oT", ec, 1)])
                wo, ko = self.wslot((4, 1024))
                self.wload(wo, w_o[:, h * 4:(h + 1) * 4, :], ko)
                for n in range(8):
                    for tb in range(2):
                        bk, bkey = self.bank()
                        for ec in range(4):
                            self.mm(bk, wo[:, ec, n * 128:(n + 1) * 128], oT[:, ec, tb * 512:(tb + 1) * 512], ec == 0, ec == 3,
                                    [ko, ("oT", ec, tb)], [bkey])
                        self.h_add(bk, bkey, n, hf, tb)

    def norm_sumsq(self, osq, okey, c0):
        bN, kN = self.bank()
        for ec in range(4):
            self.mm(bN[:, 0:128], self.ones[:], osq[:, ec, :], ec == 0, ec == 3, [okey, "ones"], [kN])
        msq = self.msq
        self.op("dve", lambda e: e.tensor_copy(out=msq[:, c0:c0 + RC], in_=bN[:, 0:128]), reads=[kN], writes=["msq"])

    def alloc_ffn(self, moe):
        self.nslots = 6
        self.wslots = [self.sb("wslot", [128, 4096], BF16) for _ in range(self.nslots)]
        self.alloc_norm()
        self.xn = self.sb("xn", [128, 8, TT], BF16)
        self.act = [self.sb("act", [128, 4, TT], BF16) for _ in range(2)]
        self.sg = [self.sb("sg", [128, 512], F32) for _ in range(2)]
        if moe:
            self.ug = [self.sb("ug", [128, 512], F32) for _ in range(2)]
            self.gateB = self.sb("gateB", [128, 8, TT], F32)
            self.lg = self.sb("lg", [128, 8], F32)
            self.m8 = self.sb("m8", [128, 8], F32)
            self.gw = self.sb("gw", [128, 4], F32)
            self.gate = self.sb("gate", [128, 8], F32)
            self.g1 = self.sb("g1", [128, 8], F32)
            self.diag = [self.sb("diag", [128, 128], F32) for _ in range(2)]
            self.onesf = self.sb("onesf", [128, 128], F32)
            self.wr = self.sb("wr", [128, 8, 8], BF16)

    def ffn_expert(self, hf, wg_d, wu_d, wd_d, F, pw, e_idx):
        op = self.op
        xn = self.xn
        npan = F // pw
        nfc = pw // 128
        cnt = 0
        for j in range(npan):
            wg, kg = self.wslot((8, pw))
            self.wload(wg, wg_d[:, :, j * pw:(j + 1) * pw], kg)
            wu, ku = self.wslot((8, pw))
            self.wload(wu, wu_d[:, :, j * pw:(j + 1) * pw], ku)
            wd, kd = self.wslot((nfc, 1024))
            self.wload(wd, wd_d[:, j * nfc:(j + 1) * nfc, :], kd)
            act = self.act[j % 2]
            akey = ("act", j % 2)
            for fc in range(nfc):
                for tb in range(2):
                    bg, kgb = self.bank()
                    bu, kub = self.bank()
                    for k in range(8):
                        self.mm(bg, wg[:, k, fc * 128:(fc + 1) * 128], xn[:, k, tb * 512:(tb + 1) * 512], k == 0, k == 7, [kg, ("xn", k, tb)], [kgb])
                    for k in range(8):
                        self.mm(bu, wu[:, k, fc * 128:(fc + 1) * 128], xn[:, k, tb * 512:(tb + 1) * 512], k == 0, k == 7, [ku, ("xn", k, tb)], [kub])
                    sg = self.sg[cnt % 2]
                    skey = ("sg", cnt % 2)
                    op("act", lambda e, sg=sg, bg=bg: e.activation(out=sg[:], in_=bg, func=AF.Silu), reads=[kgb], writes=[skey])
                    a_out = act[:, fc, tb * 512:(tb + 1) * 512]
                    if e_idx is None:
                        op("dve", lambda e, a_out=a_out, sg=sg, bu=bu: e.tensor_tensor(out=a_out, in0=bu, in1=sg[:], op=ALU.mult),
                           reads=[kub, skey], writes=[akey + (fc, tb)])
                    else:
                        ug = self.ug[cnt % 2]
                        ukey = ("ug", cnt % 2)
                        gb = self.gateB[:, e_idx, tb * 512:(tb + 1) * 512]
                        op("dve", lambda e, ug=ug, bu=bu, gb=gb: e.tensor_tensor(out=ug[:], in0=bu, in1=gb, op=ALU.mult),
                           reads=[kub, "gateB"], writes=[ukey])
                        op("dve", lambda e, a_out=a_out, sg=sg, ug=ug: e.tensor_tensor(out=a_out, in0=ug[:], in1=sg[:], op=ALU.mult),
                           reads=[ukey, skey], writes=[akey + (fc, tb)])
                    cnt += 1
            for n in range(8):
                for tb in range(2):
                    bk, bkey = self.bank()
                    for fc in range(nfc):
                        self.mm(bk, wd[:, fc, n * 128:(n + 1) * 128], act[:, fc, tb * 512:(tb + 1) * 512], fc == 0, fc == nfc - 1,
                                [kd, akey + (fc, tb)], [bkey])
                    self.h_add(bk, bkey, n, hf, tb)

    def dense_ffn(self):
        self.set_banks(list(range(8)), [])
        for hf in range(2):
            self.norm_to_xn(hf, VEC_FFN)
            self.ffn_expert(hf, self.wg_dense, self.wu_dense, self.wd_dense, 2816, 256, None)

    def router(self, hf):
        op = self.op
        xn = self.xn
        wr = self.wr
        lg, m8, gw, gate, g1 = self.lg, self.m8, self.gw, self.gate, self.g1
        for tl in range(8):
            tb = tl // 4
            bk, bkey = self.bank()
            for k in range(8):
                self.mm(bk[:, 0:8], xn[:, k, tl * 128:(tl + 1) * 128], wr[:, k, :], k == 0, k == 7, ["wr", ("xn", k, tb)], [bkey])
            op("dve", lambda e, bk=bk: e.tensor_copy(out=lg[:], in_=bk[:, 0:8]), reads=[bkey], writes=["lg"])
            op("dve", lambda e: e.max(out=m8[:], in_=lg[:]), reads=["lg"], writes=["m8"])
            op("dve", lambda e: e.tensor_tensor(out=gw[:, 0:1], in0=m8[:, 0:1], in1=m8[:, 1:2], op=ALU.subtract), reads=["m8"], writes=["gw0"])
            op("act", lambda e: e.activation(out=gw[:, 1:2], in_=gw[:, 0:1], func=AF.Sigmoid), reads=["gw0"], writes=["gw1"])
            op("act", lambda e: e.activation(out=gw[:, 2:3], in_=gw[:, 0:1], func=AF.Sigmoid, scale=-1.0), reads=["gw0"], writes=["gw2"])
            op("dve", lambda e: e.tensor_scalar(out=gate[:], in0=lg[:], scalar1=m8[:, 0:1], scalar2=gw[:, 1:2], op0=ALU.is_equal, op1=ALU.mult),
               reads=["lg", "m8", "gw1"], writes=["gate"])
            op("dve", lambda e: e.tensor_scalar(out=g1[:], in0=lg[:], scalar1=m8[:, 1:2], scalar2=gw[:, 2:3], op0=ALU.is_equal, op1=ALU.mult),
               reads=["lg", "m8", "gw2"], writes=["g1"])
            op("dve", lambda e: e.tensor_tensor(out=gate[:], in0=gate[:], in1=g1[:], op=ALU.add), reads=["gate", "g1"], writes=["gate"])
            b2, keys2 = self.bank2()
            for ex in range(8):
                dg = self.diag[ex % 2]
                dk = ("diag", ex % 2)
                op("dve", lambda e, dg=dg, ex=ex: e.tensor_scalar(out=dg[:], in0=self.identf, scalar1=gate[:, ex:ex + 1], scalar2=None, op0=ALU.mult),
                   reads=["gate", "consts"], writes=[dk])
                self.mm(b2[:, ex * 128:(ex + 1) * 128], self.onesf[:], dg[:], True, True, [dk, "onesf"], [keys2[ex // 4]])
            gv = self.gateB[:, :, tl * 128:(tl + 1) * 128]
            op("act", lambda e, gv=gv, b2=b2: e.activation(out=gv, in_=b2[:, :].rearrange("p (a b) -> p a b", a=8), func=AF.Copy),
               reads=keys2, writes=["gateB"])

    def moe_ffn(self):
        op = self.op
        self.set_banks([0, 1, 2, 3, 4, 5], [3])
        op("dve", lambda e: e.memset(self.onesf[:], 1.0), writes=["onesf"])
        wr_d = self.w_router
        op("pool", lambda e: e.dma_start(out=self.wr[:], in_=wr_d), writes=["wr"], dsem="wr")
        for hf in range(2):
            self.norm_to_xn(hf, VEC_FFN + 8)
            self.set_banks([0, 1, 2, 3, 4, 5], [3])
            self.router(hf)
            self.set_banks(list(range(8)), [])
            for ex in range(8):
                self.ffn_expert(hf, self.wg_moe[ex], self.wu_moe[ex], self.wd_moe[ex], 3584, 512, ex)

    def alloc_ple(self):
        self.nslots = 4
        self.wslots = [self.sb("wslot", [128, 4096], BF16) for _ in range(self.nslots)]
        self.alloc_norm()
        self.xn = self.sb("xn", [128, 8, TT], BF16)
        self.pbf = self.sb("pbf", [128, 2, TT], BF16)
        self.sg = [self.sb("sg", [128, 512], F32) for _ in range(2)]
        self.pt = [self.sb("pt", [128, 512], F32) for _ in range(2)]

    def ple(self, layer):
        op = self.op
        self.set_banks(list(range(8)), [])
        xn, hT = self.xn, self.hT
        wup_d = self.w_ple_up[layer]
        wgt_d = self.w_ple_gate[layer]
        cnt = 0
        for hf in range(2):
            self.norm_to_xn(hf, VEC_PLE + 8 * layer)
            pbf = self.pbf
            op("pool", lambda e, hf=hf: e.dma_start(out=pbf[:], in_=self.pT_d[:, layer, :, hf * TT:(hf + 1) * TT]),
               writes=["pbf"], dsem="pbf")
            wup, kup = self.wslot((2, 1024))
            self.wload(wup, wup_d, kup)
            for pn in range(2):
                wgt, kgt = self.wslot((8, 512))
                self.wload(wgt, wgt_d[:, :, pn * 512:(pn + 1) * 512], kgt)
                for nn in range(4):
                    n = pn * 4 + nn
                    for tb in range(2):
                        ba, ka = self.bank()
                        bb, kb = self.bank()
                        for kc in range(2):
                            self.mm(ba, wup[:, kc, n * 128:(n + 1) * 128], pbf[:, kc, tb * 512:(tb + 1) * 512], kc == 0, kc == 1, [kup, "pbf"], [ka])
                        for k in range(8):
                            self.mm(bb, wgt[:, k, nn * 128:(nn + 1) * 128], xn[:, k, tb * 512:(tb + 1) * 512], k == 0, k == 7, [kgt, ("xn", k, tb)], [kb])
                        sg = self.sg[cnt % 2]
                        pt = self.pt[cnt % 2]
                        op("act", lambda e, sg=sg, bb=bb: e.activation(out=sg[:], in_=bb, func=AF.Sigmoid), reads=[kb], writes=[("sg", cnt % 2)])
                        op("dve", lambda e, pt=pt, ba=ba, sg=sg: e.tensor_tensor(out=pt[:], in0=ba, in1=sg[:], op=ALU.mult),
                           reads=[ka, ("sg", cnt % 2)], writes=[("pt", cnt % 2)])
                        c0 = hf * TT + tb * 512
                        op("dve", lambda e, pt=pt, n=n, c0=c0: e.tensor_tensor(out=hT[:, n, c0:c0 + 512], in0=pt[:], in1=hT[:, n, c0:c0 + 512], op=ALU.add),
                           reads=[("pt", cnt % 2), ("h", n, hf, tb)], writes=[("h", n, hf, tb)])
                        cnt += 1

    def alloc_kv(self):
        self.nslots = 4
        self.wslots = [self.sb("wslot", [128, 4096], BF16) for _ in range(self.nslots)]
        self.alloc_norm()
        self.xn = self.sb("xn", [128, 8, TT], BF16)
        self.kst = [self.sb("kst", [128, TT], BF16) for _ in range(2)]
        self.vst = self.sb("vst", [128, 8, 16, 65], BF16)

    def kv_proj(self, kT_dst, v_dst, slab):
        op = self.op
        self.set_banks(list(range(8)), [])
        xn = self.xn
        vst = self.vst
        w_kv = self.w_kv
        op("dve", lambda e: e.memset(vst[:, :, :, 64:65], 1.0), writes=["vst1"])
        for hf in range(2):
            self.norm_to_xn(hf, VEC_KV)
            for pn in range(2):
                wk, kk = self.wslot((8, 512))
                self.wload(wk, w_kv[:, :, pn * 512:(pn + 1) * 512], kk)
                for nn in range(4):
                    n = pn * 4 + nn
                    kst = self.kst[n % 2]
                    for tb in range(2):
                        bk, bkey = self.bank()
                        for k in range(8):
                            self.mm(bk, wk[:, k, nn * 128:(nn + 1) * 128], xn[:, k, tb * 512:(tb + 1) * 512], k == 0, k == 7, [kk, ("xn", k, tb)], [bkey])
                        op("act", lambda e, kst=kst, bk=bk, tb=tb: e.activation(out=kst[:, tb * 512:(tb + 1) * 512], in_=bk, func=AF.Copy),
                           reads=[bkey], writes=[("kst", n % 2, tb)])
                    op("sp", lambda e, kst=kst, n=n, hf=hf: e.dma_start(out=kT_dst(n, hf * TT, TT), in_=kst[:]),
                       reads=[("kst", n % 2, 0), ("kst", n % 2, 1)], writes=[("kTd", n, hf)], dsem="kst%d" % (n % 2))
                    if hf == 1 and slab is not None:
                        op("sp", lambda e, kst=kst, n=n: e.dma_start(out=slab[:, n * 512:(n + 1) * 512], in_=kst[:, 512:1024]),
                           reads=[("kst", n % 2, 1)], writes=[("slabk", n)], dsem="kst%d" % (n % 2))
            for vp in range(2):
                wv, kvk = self.wslot((8, 512))
                self.wload(wv, w_kv[:, :, 1024 + vp * 512:1024 + (vp + 1) * 512], kvk)
                for tl in range(8):
                    tb = tl // 4
                    bk, bkey = self.bank()
                    for k in range(8):
                        self.mm(bk, xn[:, k, tl * 128:(tl + 1) * 128], wv[:, k, :], k == 0, k == 7, [kvk, ("xn", k, tb)], [bkey])
                    op("act", lambda e, bk=bk, tl=tl, vp=vp: e.activation(out=vst[:, tl, vp * 8:(vp + 1) * 8, 0:64],
                                                                            in_=bk.rearrange("p (a b) -> p a b", a=8), func=AF.Copy),
                       reads=[bkey, "vst1"], writes=[("vst", tl, vp)])
            op("sp", lambda e, hf=hf: e.dma_start(out=v_dst(hf * 8, 8), in_=vst[:].rearrange("p a b c -> p a (b c)")),
               reads=[("vst", tl, vp) for tl in range(8) for vp in range(2)], writes=[("vd", hf)], dsem="vst")
            if hf == 1 and slab is not None:
                op("sp", lambda e: e.dma_start(out=slab[:, 4096:4096 + 4160].rearrange("p (a b) -> p a b", a=4),
                                               in_=vst[:, 4:8].rearrange("p a b c -> p a (b c)")),
                   reads=[("vst", tl, vp) for tl in range(4, 8) for vp in range(2)], writes=["slabv"], dsem="vst")

    def alloc_l1mix(self):
        self.nslots = 3
        self.wslots = [self.sb("wslot", [128, 4096], BF16) for _ in range(self.nslots)]
        self.alloc_norm()
        self.xn = self.sb("xn", [128, 8, TT], BF16)
        self.qT = self.sb("qT", [128, 8, TT], BF16)
        self.oT1 = self.sb("oT1", [128, 8, TT], BF16)
        self.biasT = self.sb("biasT", [128, 16, 5, 128], BF16)
        self.Kc = [self.sb("Kc", [128, 1536], BF16) for _ in range(2)]
        self.Vc = [self.sb("Vc", [128, 12, 130], BF16) for _ in range(2)]
        self.eT = [self.sb("eT", [128, 640], BF16) for _ in range(2)]
        self.otok = [self.sb("otok", [128, 128], BF16) for _ in range(2)]
        self.rden = [self.sb("rden", [128, 1], F32) for _ in range(2)]

    def l1_mixer(self, kT_src, v_src):
        op = self.op
        xn, qT, oT1, hT = self.xn, self.qT, self.oT1, self.hT
        biasT = self.biasT
        for hg in range(4):
            op("pool", lambda e, hg=hg: e.dma_start(out=biasT[:, hg * 4:(hg + 1) * 4].rearrange("p a b c -> p a (b c)"),
                                                   in_=self.bias_d[:, hg * 4:(hg + 1) * 4].rearrange("p a b c -> p a (b c)")),
               writes=[("biasT", hg)], dsem="bias%d" % hg)
        wq_d, wo_d = self.w_q_b, self.w_o_b
        cnt = 0
        for hf in range(2):
            self.set_banks([0, 1, 2, 3], [2, 3])
            self.norm_to_xn(hf, VEC_MIX + 8)
            for pn in range(2):
                wq, kq = self.wslot((8, 512))
                self.wload(wq, wq_d[:, :, pn * 512:(pn + 1) * 512], kq)
                for nn in range(4):
                    n = pn * 4 + nn
                    for tb in range(2):
                        bk, bkey = self.bank()
                        for k in range(8):
                            self.mm(bk, wq[:, k, nn * 128:(nn + 1) * 128], xn[:, k, tb * 512:(tb + 1) * 512], k == 0, k == 7, [kq, ("xn", k, tb)], [bkey])
                        op("act", lambda e, bk=bk, n=n, tb=tb: e.activation(out=qT[:, n, tb * 512:(tb + 1) * 512], in_=bk, func=AF.Copy, scale=0.125),
                           reads=[bkey], writes=[("qT", n, tb)])
            for hp in range(8):
                Kc = self.Kc[hp % 2]
                Vc = self.Vc[hp % 2]
                op("sp", lambda e, Kc=Kc, hp=hp, hf=hf: e.dma_start(out=Kc[:], in_=kT_src[:, hp, hf * TT:hf * TT + 1536]),
                   writes=[("Kc", hp % 2)], dsem="Kc%d" % (hp % 2))
                op("sp", lambda e, Vc=Vc, hp=hp, hf=hf: e.dma_start(out=Vc[:], in_=v_src[:, hf * 8:hf * 8 + 12, hp * 130:(hp + 1) * 130]),
                   writes=[("Vc", hp % 2)], dsem="Vc%d" % (hp % 2))
                for cp in range(8):
                    tb = cp // 4
                    otok = self.otok[cp % 2]
                    for hh in range(2):
                        h = hp * 2 + hh
                        r0 = hh * 64
                        b2, keys2 = self.bank2()
                        for kb in range(5):
                            oc = b2[:, kb * 128:(kb + 1) * 128]
                            kcol = cp * 128 + kb * 128
                            self.mm(oc, Kc[r0:r0 + 64, kcol:kcol + 128], qT[r0:r0 + 64, hp, cp * 128:(cp + 1) * 128], True, False,
                                    [("Kc", hp % 2), ("qT", hp, tb)], [keys2[kb // 4]])
                            self.mm(oc, self.ident[:], biasT[:, h, kb, :], False, True, [("biasT", h // 4), "ident"], [keys2[kb // 4]])
                        eT = self.eT[cnt % 2]
                        ekey = ("eT", cnt % 2)
                        op("act", lambda e, eT=eT, b2=b2: e.activation(out=eT[:], in_=b2[:, 0:640], func=AF.Exp), reads=keys2, writes=[ekey])
                        bO, kO = self.bank()
                        for kb in range(5):
                            self.mm(bO[:, 0:65], eT[:, kb * 128:(kb + 1) * 128], Vc[:, cp + kb, hh * 65:(hh + 1) * 65], kb == 0, kb == 4,
                                    [ekey, ("Vc", hp % 2)], [kO])
                        rd = self.rden[cnt % 2]
                        rkey = ("rden", cnt % 2)
                        op("dve", lambda e, rd=rd, bO=bO: e.reciprocal(out=rd[:], in_=bO[:, 64:65]), reads=[kO], writes=[rkey])
                        op("dve", lambda e, rd=rd, bO=bO, otok=otok, hh=hh: e.tensor_scalar(out=otok[:, hh * 64:(hh + 1) * 64], in0=bO[:, 0:64],
                                                                                             scalar1=rd[:], scalar2=None, op0=ALU.mult),
                           reads=[kO, rkey], writes=[("otok", cp % 2, hh)])
                        cnt += 1
                    bT, kTb = self.bank()
                    bTb = bT[:, 0:64].bitcast(BF16)
                    op("pe", lambda e, bTb=bTb, otok=otok: e.transpose(bTb, otok[:], self.ident[:]),
                       reads=[("otok", cp % 2, 0), ("otok", cp % 2, 1), "ident"], writes=[kTb])
                    op("act", lambda e, bTb=bTb, hp=hp, cp=cp: e.activation(out=oT1[:, hp, cp * 128:(cp + 1) * 128], in_=bTb, func=AF.Copy),
                       reads=[kTb], writes=[("oT1", hp, tb)])
            for pn in range(2):
                wo, ko = self.wslot((8, 512))
                self.wload(wo, wo_d[:, :, pn * 512:(pn + 1) * 512], ko)
                for nn in range(4):
                    n = pn * 4 + nn
                    for tb in range(2):
                        bk, bkey = self.bank()
                        for k in range(8):
                            self.mm(bk, wo[:, k, nn * 128:(nn + 1) * 128], oT1[:, k, tb * 512:(tb + 1) * 512], k == 0, k == 7, [ko, ("oT1", k, tb)], [bkey])
                        self.h_add(bk, bkey, n, hf, tb)

    def final_norm(self, out_d):
        self.alloc_norm()
        self.fo = [self.sb("fo", [128, 8, 512], F32) for _ in range(2)]
        self.set_banks(list(range(8)), [])
        i = 0
        for hf in range(2):
            hT, sq, rstd, vecs, ones = self.hT, self.sq, self.rstd, self.vecs, self.ones
            for tb in range(2):
                fo = self.fo[i % 2]
                fkey = ("fo", i % 2)
                c0 = hf * TT + tb * 512
                self.op("act", lambda e, c0=c0: e.activation(out=sq[:], in_=hT[:, :, c0:c0 + 512], func=AF.Square),
                        reads=[("h", k, hf, tb) for k in range(8)], writes=["sq"])
                bk, bkey = self.bank()
                for k in range(8):
                    self.mm(bk, ones[:], sq[:, k, :], k == 0, k == 7, ["sq", "ones"], [bkey])
                rs = rstd[:, tb * 512:(tb + 1) * 512]
                self.op("act", lambda e, bk=bk, rs=rs: e.activation(out=rs, in_=bk, func=AF.Sqrt, scale=1.0 / D, bias=EPS),
                        reads=[bkey], writes=[("rstd", tb)])
                self.op("dve", lambda e, rs=rs: e.reciprocal(out=rs, in_=rs), reads=[("rstd", tb)], writes=[("rstd", tb)])
                for k in range(8):
                    self.op("dve", lambda e, k=k, c0=c0, fo=fo, rs=rs: e.scalar_tensor_tensor(
                        out=fo[:, k, :], in0=hT[:, k, c0:c0 + 512], scalar=vecs[:, VEC_FIN + k:VEC_FIN + k + 1], in1=rs,
                        op0=ALU.mult, op1=ALU.mult),
                        reads=[("h", k, hf, tb), ("rstd", tb), "vecs"], writes=[fkey + (k,)])
                self.op("sp", lambda e, fo=fo, c0=c0: e.dma_start(out=out_d[:, :, c0:c0 + 512], in_=fo[:]),
                        reads=[fkey + (k,) for k in range(8)], writes=[("outd", i)], dsem="fo%d" % (i % 2))
                i += 1
        self.final_dsems += ["fo0", "fo1"]

    def decl_l0_weights(self, full):
        self.pos_d = self.dram_in("pos", [1, T], I32)
        self.w_in = self.dram_in("w_in_a", [1, 1024, 6144], F32)[0].rearrange("(k p) n -> p k n", p=128)
        if full:
            self.w_o_a = self.dram_in("w_out_a", [1, 2048, 1024], F32)[0].rearrange("(k p) n -> p k n", p=128)
            self.wg_dense = self.dram_in("w_gate_dense", [1, 1024, 2816], F32)[0].rearrange("(k p) n -> p k n", p=128)
            self.wu_dense = self.dram_in("w_up_dense", [1, 1024, 2816], F32)[0].rearrange("(k p) n -> p k n", p=128)
            self.wd_dense = self.dram_in("w_down_dense", [1, 2816, 1024], F32)[0].rearrange("(k p) n -> p k n", p=128)
            self.w_kv = self.dram_in("w_kv", [1024, 2048], F32).rearrange("(k p) n -> p k n", p=128)

    def decl_ple(self):
        self.pT_d = self.dram_in("pT", [128, 2, 2, T], F32)
        wu = self.dram_in("w_ple_up", [2, 256, 1024], F32)
        wg = self.dram_in("w_ple_gate", [2, 1024, 1024], F32)
        self.w_ple_up = [wu[l].rearrange("(k p) n -> p k n", p=128) for l in range(2)]
        self.w_ple_gate = [wg[l].rearrange("(k p) n -> p k n", p=128) for l in range(2)]

    def decl_l1_weights(self):
        self.w_q_b = self.dram_in("w_q_b", [1, 1024, 1024], F32)[0].rearrange("(k p) n -> p k n", p=128)
        self.w_o_b = self.dram_in("w_out_b", [1, 1024, 1024], F32)[0].rearrange("(k p) n -> p k n", p=128)
        self.bias_d = self.dram_in("biasT", [128, 16, 5, 128], F32)
        self.w_router = self.dram_in("w_router", [1, 1024, 8], F32)[0].rearrange("(k p) n -> p k n", p=128)
        wg = self.dram_in("w_gate_moe", [1, 8, 1024, 3584], F32)[0]
        wu = self.dram_in("w_up_moe", [1, 8, 1024, 3584], F32)[0]
        wd = self.dram_in("w_down_moe", [1, 8, 3584, 1024], F32)[0]
        self.wg_moe = [wg[e].rearrange("(k p) n -> p k n", p=128) for e in range(8)]
        self.wu_moe = [wu[e].rearrange("(k p) n -> p k n", p=128) for e in range(8)]
        self.wd_moe = [wd[e].rearrange("(k p) n -> p k n", p=128) for e in range(8)]

    def build(self):
        st = self.stage
        op = self.op
        self.setup_common()
        xT_d = None
        if st in ("A", "B", "ALL"):
            xT_d = self.dram_in("xT", [128, 8, T], F32)
            self.decl_l0_weights(full=(st != "A"))
            self.load_h(xT_d)
        if st == "A":
            slab_s = self.dram_out("slab_s", [128, 4096], F32)
            self.l0_pass1()
            for h in range(4):
                op("sp", lambda e, h=h: e.dma_start(out=slab_s[:, h * 1024:(h + 1) * 1024], in_=self.state[:, h, :]),
                   reads=[("state", h)], writes=[("slab_s", h)], dsem="slab%d" % (h % 2))
            self.final_dsems += ["slab0", "slab1"]
        if st == "B":
            sall = self.dram_in("sall", [1024, 4096], F32)
            self.decl_ple()
            hT_out = self.dram_out("hT_out", [128, 8, T], F32)
            kT_out = self.dram_out("kT_out", [128, 8, T], BF16)
            v_out = self.dram_out("v_out", [128, 16, 1040], BF16)
            mark = self.off
            self.alloc_l0mix(main=True)
            self.l0_combine(sall)
            self.l0_main()
            self.phase_barrier()
            self.off = mark
            self.alloc_ffn(moe=False)
            self.dense_ffn()
            self.phase_barrier()
            self.off = mark
            self.alloc_ple()
            self.ple(0)
            self.phase_barrier()
            self.off = mark
            self.alloc_kv()
            self.kv_proj(lambda n, c0, ln: kT_out[:, n, c0:c0 + ln], lambda t0, nt: v_out[:, t0:t0 + nt, :], None)
            self.final_dsems += ["kst0", "kst1", "vst"]
            self.store_h(hT_out, "hT_out")
        if st == "C":
            hT_in = self.dram_in("hT_in", [128, 8, T], F32)
            kT_in = self.dram_in("kT_in", [128, 8, 2560], BF16)
            v_in = self.dram_in("v_in", [128, 20, 1040], BF16)
            out_d = self.dram_out("outT", [128, 8, T], F32)
            self.decl_ple()
            self.decl_l1_weights()
            self.load_h(hT_in)
            mark = self.off
            self.alloc_l1mix()
            self.l1_mixer(kT_in, v_in)
            self.phase_barrier()
            self.off = mark
            self.alloc_ffn(moe=True)
            self.moe_ffn()
            self.phase_barrier()
            self.off = mark
            self.alloc_ple()
            self.ple(1)
            self.phase_barrier()
            self.off = mark
            self.final_norm(out_d)
        self.S.emit(final_wait_dsems=sorted(set(self.final_dsems)))
        return self.nc


def _fm(a):
    Tn, Fn = a.shape
    return np.ascontiguousarray(a.T.reshape(Fn // 128, 128, Tn).transpose(1, 0, 2))


def _vec_cols(v):
    return v.reshape(-1, 128).T


def _consts():
    c = np.zeros((128, 1157), np.float64)
    j = np.arange(128)[:, None]
    i = np.arange(128)[None, :]
    for h in range(4):
        g = GAM[h]
        c[:, h * 128:(h + 1) * 128] = np.where(j <= i, g ** (-(j + 1.0)) / 16.0, 0.0)
        c[:, 512 + h * 128:512 + (h + 1) * 128] = g ** (i + 1.0)
        c[:, 1024 + h] = g ** (127.0 - np.arange(128)) / 16.0
    c[:, 1028:1156] = np.eye(128)
    half = 128
    inv_freq = (np.float32(1.0) / (np.float32(10000.0) ** np.linspace(0.0, 1.0, half, dtype=np.float32))).astype(np.float32)
    c = c.astype(np.float32)
    c[:, 1156] = inv_freq
    return c


def _coef(core):
    c = np.zeros((128, 40), np.float64)
    b, seg = core // 4, core % 4
    for src in range(8):
        sb_, ss = src // 4, src % 4
        if sb_ == b and ss < seg:
            for h in range(4):
                c[:, src * 4 + h] = GAM[h] ** (2048.0 * (seg - 1 - ss))
        if sb_ == b and ss == seg - 1:
            c[:, 32 + src] = 1.0
    return c.astype(np.float32)


def _bias_tiles(rel_bias):
    jp = np.arange(640)[:, None]
    ip = np.arange(128)[None, :]
    rel = np.clip(ip - jp + 512, -63, 256) + 63
    qa = ip < 64
    valid = np.where(qa, jp < 576, jp >= 64)
    bt = rel_bias[:, rel]
    bt = np.where(valid[None], bt, np.float32(NEG)).astype(np.float32)
    bt = bt.reshape(16, 5, 128, 128).transpose(2, 0, 1, 3)
    return np.ascontiguousarray(bt)


_PROG_CACHE = {}


def _get_prog(stage):
    if stage not in _PROG_CACHE:
        _PROG_CACHE[stage] = Builder(stage).build()
    return _PROG_CACHE[stage]


def _common_maps(inputs):
    consts = _consts()
    vecs = np.concatenate([
        _vec_cols(inputs["norm_mix"][0]), _vec_cols(inputs["norm_mix"][1]),
        _vec_cols(inputs["norm_ffn"][0]), _vec_cols(inputs["norm_ffn"][1]),
        _vec_cols(inputs["norm_ple"][0]), _vec_cols(inputs["norm_ple"][1]),
        _vec_cols(inputs["norm_kv"]), _vec_cols(inputs["norm_final"]),
        _vec_cols(inputs["ret_gn"][0]),
    ], axis=1).astype(np.float32)
    maps = []
    for c in range(NCORES):
        maps.append({"vecs": np.ascontiguousarray(vecs), "consts": consts, "coef": _coef(c)})
    return maps


def run_stage_A(inputs):
    maps = _common_maps(inputs)
    for c in range(NCORES):
        b, seg = c // 4, c % 4
        maps[c]["xT"] = _fm(inputs["x"][b, seg * T:(seg + 1) * T, :])
        maps[c]["pos"] = np.ascontiguousarray(inputs["positions"][b, seg * T:(seg + 1) * T].reshape(1, T).astype(np.int32))
        maps[c]["w_in_a"] = inputs["w_in_a"]
    res = run_bass_kernel_spmd(_get_prog("A"), maps, core_ids=list(range(NCORES)))
    return [r["slab_s"] for r in res.results]


def _ple_pT(inputs, c):
    b, seg = c // 4, c % 4
    p = inputs["p"][:, b, seg * T:(seg + 1) * T, :]
    pT = p.transpose(0, 2, 1).reshape(2, 2, 128, T).transpose(2, 0, 1, 3)
    return np.ascontiguousarray(pT)


def run_stage_B(inputs, slabs):
    maps = _common_maps(inputs)
    sall = np.ascontiguousarray(np.concatenate(slabs, axis=0))
    for c in range(NCORES):
        b, seg = c // 4, c % 4
        m = maps[c]
        m["xT"] = _fm(inputs["x"][b, seg * T:(seg + 1) * T, :])
        m["pos"] = np.ascontiguousarray(inputs["positions"][b, seg * T:(seg + 1) * T].reshape(1, T).astype(np.int32))
        m["sall"] = sall
        m["pT"] = _ple_pT(inputs, c)
        for k in ("w_in_a", "w_out_a", "w_gate_dense", "w_up_dense", "w_down_dense", "w_kv", "w_ple_up", "w_ple_gate"):
            m[k] = inputs[k]
    res = run_bass_kernel_spmd(_get_prog("B"), maps, core_ids=list(range(NCORES)))
    return res.results


def run_stage_C(inputs, resB):
    maps = _common_maps(inputs)
    bias = _bias_tiles(inputs["rel_bias"][0])
    for c in range(NCORES):
        b, seg = c // 4, c % 4
        m = maps[c]
        m["hT_in"] = resB[c]["hT_out"]
        kT = np.zeros((128, 8, 2560), ml_dtypes.bfloat16)
        v = np.zeros((128, 20, 1040), ml_dtypes.bfloat16)
        kT[:, :, 512:] = resB[c]["kT_out"]
        v[:, 4:, :] = resB[c]["v_out"]
        if seg > 0:
            kT[:, :, :512] = resB[c - 1]["kT_out"][:, :, T - 512:]
            v[:, :4, :] = resB[c - 1]["v_out"][:, 12:, :]
        m["kT_in"] = kT
        m["v_in"] = v
        m["pT"] = _ple_pT(inputs, c)
        m["biasT"] = bias
        for k in ("w_q_b", "w_out_b", "w_router", "w_gate_moe", "w_up_moe", "w_down_moe", "w_ple_up", "w_ple_gate"):
            m[k] = inputs[k]
    res = run_bass_kernel_spmd(_get_prog("C"), maps, core_ids=list(range(NCORES)))
    return res.results


def _assemble(resC):
    out = np.zeros((2, 4 * T, D), np.float32)
    for c in range(NCORES):
        b, seg = c // 4, c % 4
        oT = resC[c]["outT"]
        out[b, seg * T:(seg + 1) * T, :] = oT.transpose(2, 1, 0).reshape(T, D)
    return out


def kernel(**inputs):
    inputs = {k: np.asarray(v) for k, v in inputs.items()}
    slabs = run_stage_A(inputs)
    resB = run_stage_B(inputs, slabs)
    resC = run_stage_C(inputs, resB)
    return _assemble(resC)
```
